# Optimizing a Trainium2 kernel written in Bass

```python
import math
import jax, jax.numpy as jnp
from jax import lax
import numpy as np

D_MODEL = 2048
BATCH = 4
SEQ = 8192
DEPTH = 2

N_A_LAYERS = DEPTH // 2
N_B_LAYERS = DEPTH - N_A_LAYERS
N_DENSE_LAYERS = (DEPTH + 1) // 2
N_MOE_LAYERS = DEPTH // 2

MIX_WIDTH = D_MODEL
MEM_WIDTH = D_MODEL // 4
MAIN_WIDTH = MIX_WIDTH - MEM_WIDTH
MEM_HEADS = 4
MEM_HEAD_DIM = MEM_WIDTH // MEM_HEADS
N_MEM = 256

CHUNK = 128
SG_GROUP_DIM = 128
SG_GROUPS = MAIN_WIDTH // SG_GROUP_DIM

DIFF_QK_DIM = 128
DIFF_V_DIM = 2 * DIFF_QK_DIM
DIFF_HEADS = MAIN_WIDTH // DIFF_V_DIM
Q_BLOCK = 128

D_FF_DENSE = 5632
N_EXPERTS = 8
TOP_K = 2
D_FF_EXPERT = 7168

ALPHA = (2.0 * DEPTH) ** 0.25
BETA = (8.0 * DEPTH) ** -0.25
LN_EPS = 1e-5

A_IN_COLS = 2 * MAIN_WIDTH + MEM_WIDTH
B_IN_COLS = 2 * DIFF_HEADS * DIFF_QK_DIM + MEM_WIDTH
KV_COLS = 2 * DIFF_HEADS * DIFF_QK_DIM + DIFF_HEADS * DIFF_V_DIM

kernel_name = "yoco_gmlp_diffattn_memxattn_moe_deepnorm"


def layer_norm(x, g, b):
    xf = x.astype(jnp.float32)
    mu = jnp.mean(xf, axis=-1, keepdims=True)
    var = jnp.mean(jnp.square(xf - mu), axis=-1, keepdims=True)
    return ((xf - mu) * lax.rsqrt(var + LN_EPS) * g + b).astype(x.dtype)


def rms_norm(x, g):
    xf = x.astype(jnp.float32)
    return (xf * lax.rsqrt(jnp.mean(jnp.square(xf), axis=-1, keepdims=True) + LN_EPS) * g).astype(x.dtype)


def chunked_spatial_gating(z, vnorm_g, vnorm_b, w_s, b_s):
    bsz, seq, _ = z.shape
    u, v = z[..., :MAIN_WIDTH], z[..., MAIN_WIDTH:]
    v = layer_norm(v, vnorm_g, vnorm_b)
    vc = v.reshape(bsz, seq // CHUNK, CHUNK, SG_GROUPS, SG_GROUP_DIM)
    causal = jnp.tril(jnp.ones((CHUNK, CHUNK), dtype=bool))
    w = jnp.where(causal[None], w_s, jnp.zeros_like(w_s))
    s = jnp.einsum('gts,bnsgd->bntgd', w, vc) + jnp.transpose(b_s)[:, :, None]
    return u * s.reshape(bsz, seq, MAIN_WIDTH)


def diff_attention(q, k, v, lam, lambda_init, subln_g):
    bsz, seq = q.shape[0], q.shape[1]
    nb = seq // Q_BLOCK
    scale = DIFF_QK_DIM ** -0.5
    qb = jnp.moveaxis(q.reshape(bsz, nb, Q_BLOCK, 2, DIFF_HEADS, DIFF_QK_DIM), 1, 0)
    kpos = jnp.arange(seq)

    def one_block(args):
        qi, i = args
        s = jnp.einsum('bqmhd,bkmhd->bmhqk', qi, k).astype(jnp.float32) * scale
        qpos = i * Q_BLOCK + jnp.arange(Q_BLOCK)
        mask = kpos[None, :] <= qpos[:, None]
        s = jnp.where(mask, s, jnp.finfo(jnp.float32).min)
        p = jax.nn.softmax(s, axis=-1)
        a = p[:, 0] - lam * p[:, 1]
        return jnp.einsum('bhqk,bkhd->bqhd', a.astype(v.dtype), v)

    out = lax.map(one_block, (qb, jnp.arange(nb)))
    out = jnp.moveaxis(out, 0, 1).reshape(bsz, seq, DIFF_HEADS, DIFF_V_DIM)
    out = rms_norm(out, subln_g) * (1.0 - lambda_init)
    return out.reshape(bsz, seq, MAIN_WIDTH)


def memory_attention(q_mem, mem, w_mem_kv):
    bsz, seq, _ = q_mem.shape
    kv = mem @ w_mem_kv
    k = kv[..., :MEM_WIDTH].reshape(bsz, -1, MEM_HEADS, MEM_HEAD_DIM)
    v = kv[..., MEM_WIDTH:].reshape(bsz, -1, MEM_HEADS, MEM_HEAD_DIM)
    q = q_mem.reshape(bsz, seq, MEM_HEADS, MEM_HEAD_DIM)
    s = jnp.einsum('bshd,bmhd->bhsm', q, k).astype(jnp.float32) * (MEM_HEAD_DIM ** -0.5)
    p = jax.nn.softmax(s, axis=-1).astype(v.dtype)
    return jnp.einsum('bhsm,bmhd->bshd', p, v).reshape(bsz, seq, MEM_WIDTH)


def swiglu(x, w_gate, w_up, w_down):
    return (jax.nn.silu(x @ w_gate) * (x @ w_up)) @ w_down


def moe_swiglu(x, w_router, w_gate, w_up, w_down):
    bsz, seq, d = x.shape
    xf = x.reshape(-1, d)
    logits = (xf @ w_router).astype(jnp.float32)
    top_v, top_i = lax.top_k(logits, TOP_K)
    top_w = jax.nn.softmax(top_v, axis=-1)
    gates = jnp.sum(jax.nn.one_hot(top_i, N_EXPERTS, dtype=jnp.float32) * top_w[..., None], axis=1)
    gates = gates.astype(x.dtype)
    y = jnp.zeros_like(xf)
    for e in range(N_EXPERTS):
        y = y + gates[:, e:e + 1] * swiglu(xf, w_gate[e], w_up[e], w_down[e])
    return y.reshape(bsz, seq, d)


def setup_inputs(seed: int = 0) -> dict:
    key = jax.random.key(seed)
    ks = jax.random.split(key, 32)
    f32 = jnp.float32
    d = D_MODEL

    def nrm(k, shape, scale):
        return jax.random.normal(k, shape, f32) * scale

    w_mem_kv = jnp.concatenate([
        nrm(ks[4], (DEPTH, d, MEM_WIDTH), d ** -0.5),
        nrm(ks[5], (DEPTH, d, MEM_WIDTH), d ** -0.5 * BETA)], axis=-1)
    shared_w_kv = jnp.concatenate([
        nrm(ks[11], (d, 2 * DIFF_HEADS * DIFF_QK_DIM), d ** -0.5),
        nrm(ks[12], (d, DIFF_HEADS * DIFF_V_DIM), d ** -0.5 * BETA)], axis=-1)
    return {
        "x": nrm(ks[0], (BATCH, SEQ, d), 1.0),
        "mem": nrm(ks[1], (BATCH, N_MEM, d), 1.0),
        "ln_g": 1.0 + nrm(ks[2], (DEPTH, 2, d), 0.02),
        "ln_b": nrm(ks[3], (DEPTH, 2, d), 0.02),
        "w_mix_out": nrm(ks[6], (DEPTH, MIX_WIDTH, d), MIX_WIDTH ** -0.5 * BETA),
        "w_mem_kv": w_mem_kv,
        "a_w_in": nrm(ks[7], (N_A_LAYERS, d, A_IN_COLS), d ** -0.5),
        "a_vnorm_g": 1.0 + nrm(ks[8], (N_A_LAYERS, MAIN_WIDTH), 0.02),
        "a_vnorm_b": nrm(ks[9], (N_A_LAYERS, MAIN_WIDTH), 0.02),
        "a_w_s": nrm(ks[10], (N_A_LAYERS, SG_GROUPS, CHUNK, CHUNK), CHUNK ** -0.5),
        "a_b_s": 1.0 + nrm(ks[13], (N_A_LAYERS, SG_GROUPS, CHUNK), 0.02),
        "shared_w_kv": shared_w_kv,
        "b_w_in": nrm(ks[14], (N_B_LAYERS, d, B_IN_COLS), d ** -0.5),
        "b_lambda_q1": nrm(ks[15], (N_B_LAYERS, DIFF_QK_DIM), 0.1),
        "b_lambda_k1": nrm(ks[16], (N_B_LAYERS, DIFF_QK_DIM), 0.1),
        "b_lambda_q2": nrm(ks[17], (N_B_LAYERS, DIFF_QK_DIM), 0.1),
        "b_lambda_k2": nrm(ks[18], (N_B_LAYERS, DIFF_QK_DIM), 0.1),
        "b_subln_g": 1.0 + nrm(ks[19], (N_B_LAYERS, DIFF_V_DIM), 0.02),
        "ffn_w_gate": nrm(ks[20], (N_DENSE_LAYERS, d, D_FF_DENSE), d ** -0.5),
        "ffn_w_up": nrm(ks[21], (N_DENSE_LAYERS, d, D_FF_DENSE), d ** -0.5 * BETA),
        "ffn_w_down": nrm(ks[22], (N_DENSE_LAYERS, D_FF_DENSE, d), D_FF_DENSE ** -0.5 * BETA),
        "moe_w_router": nrm(ks[23], (N_MOE_LAYERS, d, N_EXPERTS), d ** -0.5),
        "moe_w_gate": nrm(ks[24], (N_MOE_LAYERS, N_EXPERTS, d, D_FF_EXPERT), d ** -0.5),
        "moe_w_up": nrm(ks[25], (N_MOE_LAYERS, N_EXPERTS, d, D_FF_EXPERT), d ** -0.5 * BETA),
        "moe_w_down": nrm(ks[26], (N_MOE_LAYERS, N_EXPERTS, D_FF_EXPERT, d), D_FF_EXPERT ** -0.5 * BETA),
    }


def reference(x, mem, ln_g, ln_b, w_mix_out, w_mem_kv, a_w_in, a_vnorm_g, a_vnorm_b, a_w_s, a_b_s,
              shared_w_kv, b_w_in, b_lambda_q1, b_lambda_k1, b_lambda_q2, b_lambda_k2, b_subln_g,
              ffn_w_gate, ffn_w_up, ffn_w_down, moe_w_router, moe_w_gate, moe_w_up, moe_w_down):
    bsz, seq, _ = x.shape
    qk_cols = 2 * DIFF_HEADS * DIFF_QK_DIM
    k_sh = None
    v_sh = None
    for l in range(DEPTH):
        if l < N_A_LAYERS:
            a = l
            h = x @ a_w_in[a]
            z = jax.nn.gelu(h[..., :2 * MAIN_WIDTH])
            main = chunked_spatial_gating(z, a_vnorm_g[a], a_vnorm_b[a], a_w_s[a], a_b_s[a])
            q_mem = h[..., 2 * MAIN_WIDTH:]
        else:
            bi = l - N_A_LAYERS
            if bi == 0:
                kv = x @ shared_w_kv
                k_sh = kv[..., :qk_cols].reshape(bsz, seq, 2, DIFF_HEADS, DIFF_QK_DIM)
                v_sh = kv[..., qk_cols:].reshape(bsz, seq, DIFF_HEADS, DIFF_V_DIM)
            h = x @ b_w_in[bi]
            q = h[..., :qk_cols].reshape(bsz, seq, 2, DIFF_HEADS, DIFF_QK_DIM)
            lambda_init = 0.8 - 0.6 * math.exp(-0.3 * l)
            lam = (jnp.exp(jnp.sum(b_lambda_q1[bi] * b_lambda_k1[bi]).astype(jnp.float32))
                   - jnp.exp(jnp.sum(b_lambda_q2[bi] * b_lambda_k2[bi]).astype(jnp.float32))
                   + lambda_init)
            main = diff_attention(q, k_sh, v_sh, lam, lambda_init, b_subln_g[bi])
            q_mem = h[..., qk_cols:]
        mem_out = memory_attention(q_mem, mem, w_mem_kv[l])
        mix = jnp.concatenate([main, mem_out], axis=-1) @ w_mix_out[l]
        x = layer_norm(ALPHA * x + mix, ln_g[l, 0], ln_b[l, 0])
        if l % 2 == 0:
            j = l // 2
            f = swiglu(x, ffn_w_gate[j], ffn_w_up[j], ffn_w_down[j])
        else:
            j = l // 2
            f = moe_swiglu(x, moe_w_router[j], moe_w_gate[j], moe_w_up[j], moe_w_down[j])
        x = layer_norm(ALPHA * x + f, ln_g[l, 1], ln_b[l, 1])
    return x
```

```python
import math
import os
STOP = float(os.environ.get('KSTOP', '99'))
from contextlib import ExitStack

import numpy as np
import concourse.bass as bass
import concourse.mybir as mybir
from concourse.bass_utils import run_bass_kernel_spmd

F32 = mybir.dt.float32
BF16 = mybir.dt.bfloat16
AF = mybir.ActivationFunctionType
ALU = mybir.AluOpType

D = 2048
NCH = 16
T = 512
SEQ = 8192
NB = 4
NT_CORE = 8
TOK_CORE = NT_CORE * T
GT = [[0, 3, 4, 7, 8, 11, 12, 15], [1, 2, 5, 6, 9, 10, 13, 14]]
MAINW = 1536
DFF = 5632
DFE = 7168
NE = 8
ALPHA = (2.0 * 2) ** 0.25
EPS = 1e-5
GELU = AF.Gelu


class Tok:
    __slots__ = ("w", "r", "excl")

    def __init__(self, excl=False):
        self.w = None
        self.r = {}
        self.excl = excl


class Src:
    __slots__ = ("sem", "count", "name")

    def __init__(self, sem, name):
        self.sem = sem
        self.count = 0
        self.name = name


class Eng:
    def __init__(self, h, src, name):
        self.h = h
        self.src = src
        self.name = name
        self.seen = {}


class PB:
    def __init__(self, nc, n_dma_sems=40):
        self.nc = nc
        mk = lambda n: Src(nc.alloc_semaphore("s_" + n), n)
        self.pe = Eng(nc.tensor, mk("pe"), "pe")
        self.dve = Eng(nc.vector, mk("dve"), "dve")
        self.act = Eng(nc.scalar, mk("act"), "act")
        self.pool = Eng(nc.gpsimd, mk("pool"), "pool")
        self.sp = Eng(nc.sync, mk("sp"), "sp")
        self.engs = [self.pe, self.dve, self.act, self.pool, self.sp]
        self.dsems = [mk("d%d" % i) for i in range(n_dma_sems)]
        self.dpool = {id(self.pool): self.dsems[:16], id(self.sp): self.dsems[16:]}
        self.di = {id(self.pool): 0, id(self.sp): 0}
        self.banks = [nc.alloc_psum_tensor("bank%d" % i, [128, 512], F32) for i in range(8)]
        self.btok = [Tok(excl=True) for _ in range(8)]
        self.bi = 0

    def _deps(self, eng, R, W):
        deps = {}
        for t in R:
            if t.w is not None and deps.get(t.w[0], 0) < t.w[1]:
                deps[t.w[0]] = t.w[1]
        for t in W:
            if t.w is not None and deps.get(t.w[0], 0) < t.w[1]:
                deps[t.w[0]] = t.w[1]
            for s, c in t.r.items():
                if deps.get(s, 0) < c:
                    deps[s] = c
        for s, c in deps.items():
            if s is eng.src and eng is self.pe:
                continue
            if eng.seen.get(s, 0) >= c:
                continue
            eng.h.wait_ge(s.sem, c)
            eng.seen[s] = c

    def op(self, eng, fn, R=(), W=(), inc=True):
        if any(t.excl for t in R):
            W = list(W) + [t for t in R if t.excl]
            R = [t for t in R if not t.excl]
        self._deps(eng, R, W)
        ins = fn()
        tgt = eng.src.count + 1
        if inc:
            ins.then_inc(eng.src.sem, 1)
            eng.src.count = tgt
        for t in W:
            t.w = (eng.src, tgt)
            t.r = {}
        for t in R:
            if t.r.get(eng.src, 0) < tgt:
                t.r[eng.src] = tgt
        return ins

    def dma(self, q, out, in_, R=(), W=()):
        self._deps(q, R, W)
        pool_ = self.dpool[id(q)]
        ds = pool_[self.di[id(q)]]
        self.di[id(q)] = (self.di[id(q)] + 1) % len(pool_)
        if q.seen.get(ds, 0) < ds.count:
            q.h.wait_ge(ds.sem, ds.count)
            q.seen[ds] = ds.count
        q.h.dma_start(out=out, in_=in_).then_inc(ds.sem, 16)
        ds.count += 16
        for t in W:
            t.w = (ds, ds.count)
            t.r = {}
        for t in R:
            t.r[ds] = ds.count

    def ps(self):
        i = self.bi
        self.bi = (self.bi + 1) % 8
        return self.banks[i], self.btok[i]

    def barrier(self):
        srcs = [e.src for e in self.engs] + self.dsems
        for e in self.engs:
            for s in srcs:
                if s is e.src or s.count == 0:
                    continue
                if e.seen.get(s, 0) < s.count:
                    e.h.wait_ge(s.sem, s.count)
                    e.seen[s] = s.count

    def finish(self):
        e = self.sp
        for s in [x.src for x in self.engs if x is not e] + self.dsems:
            if s.count and e.seen.get(s, 0) < s.count:
                e.h.wait_ge(s.sem, s.count)
                e.seen[s] = s.count

    def mm(self, out, lhsT, rhs, start, stop, R=(), W=(), inc=None):
        if inc is None:
            inc = stop
        return self.op(self.pe, lambda: self.nc.tensor.matmul(out, lhsT=lhsT, rhs=rhs, start=start, stop=stop),
                       R=R, W=W, inc=inc)

    def tr(self, out, in_, ident, R=(), W=(), inc=True):
        return self.op(self.pe, lambda: self.nc.tensor.transpose(out, in_, ident), R=R, W=W, inc=inc)

    def actf(self, out, in_, func, R=(), W=(), **kw):
        return self.op(self.act, lambda: self.nc.scalar.activation(out=out, in_=in_, func=func, **kw), R=R, W=W)

    def v(self, fn, R=(), W=()):
        return self.op(self.dve, fn, R=R, W=W)

    def g(self, fn, R=(), W=()):
        return self.op(self.pool, fn, R=R, W=W)


_UID = [0]


class Buf:
    def __init__(self, pb, stack, name, shape, dtype, ntok=1):
        _UID[0] += 1
        self.t = stack.enter_context(pb.nc.sbuf_tensor("%s_%d" % (name, _UID[0]), shape, dtype))
        self.k = [Tok() for _ in range(ntok)]


class Ring:
    def __init__(self, pb, stack, n, name="wr", shape=(128, 16, 512)):
        self.pb = pb
        self.slabs = [Buf(pb, stack, "%s%d" % (name, i), list(shape), BF16) for i in range(n)]
        self.i = 0

    def load(self, src_ap, kc=16, ncol=512, R=()):
        b = self.slabs[self.i]
        self.i = (self.i + 1) % len(self.slabs)
        self.pb.dma(self.pb.pool, b.t[:, 0:kc, 0:ncol], src_ap, R=R, W=b.k)
        return b


def wview(w2d):
    return w2d.rearrange("(k p) n -> p k n", p=128)


class Consts:
    pass


def setup_consts(pb, stack, io):
    nc = pb.nc
    c = Consts()
    c.ident = Buf(pb, stack, "ident", [128, 128], F32)
    pb.g(lambda: nc.gpsimd.memset(c.ident.t[:], 0.0), W=c.ident.k)
    pb.g(lambda: nc.gpsimd.affine_select(out=c.ident.t[:], in_=c.ident.t[:], pattern=[[-1, 128]],
                                         compare_op=ALU.not_equal, fill=1.0, base=0, channel_multiplier=1),
         R=c.ident.k, W=c.ident.k)
    c.ones_f = Buf(pb, stack, "ones_f", [128, 128], F32)
    pb.g(lambda: nc.gpsimd.memset(c.ones_f.t[:], 1.0 / D), W=c.ones_f.k)
    c.eps = Buf(pb, stack, "eps", [128, 1], F32)
    pb.g(lambda: nc.gpsimd.memset(c.eps.t[:], EPS), W=c.eps.k)
    c.ones_b = Buf(pb, stack, "ones_b", [128, 128], BF16)
    pb.g(lambda: nc.gpsimd.memset(c.ones_b.t[:], 1.0), W=c.ones_b.k)
    c.lng = Buf(pb, stack, "lng", [128, 4, 16], F32)
    c.lnb = Buf(pb, stack, "lnb", [128, 4, 16], F32)
    with nc.allow_non_contiguous_dma(reason="tiny param loads"):
        pb.dma(pb.sp, c.lng.t[:], io["ln_g"].rearrange("l i (k p) -> p (l i) k", p=128), W=c.lng.k)
        pb.dma(pb.sp, c.lnb.t[:], io["ln_b"].rearrange("l i (k p) -> p (l i) k", p=128), W=c.lnb.k)
    return c


def ln_fm(pb, c, sc, r, li, out_bf=None, dram_out=None):
    nc = pb.nc
    ps1, k1 = pb.ps()
    ps2, k2 = pb.ps()
    for k in range(NCH):
        sq = sc["sq"][k % 2]
        pb.actf(sq.t[:], r.t[:, k, :], AF.Square, R=[r.k[k]], W=sq.k)
        pb.mm(ps1[:], c.ones_f.t[:], r.t[:, k, :], k == 0, k == NCH - 1, R=[r.k[k], c.ones_f.k[0]], W=[k1], inc=True)
        pb.mm(ps2[:], c.ones_f.t[:], sq.t[:], k == 0, k == NCH - 1, R=[sq.k[0]], W=[k2], inc=True)
    mean = sc["mean"]
    rstd = sc["rstd"]
    tmp = sc["tmp"]
    pb.v(lambda: nc.vector.tensor_copy(out=mean.t[:], in_=ps1[:]), R=[k1], W=mean.k)
    pb.v(lambda: nc.vector.tensor_tensor(out=tmp.t[:], in0=mean.t[:], in1=mean.t[:], op=ALU.mult), R=mean.k, W=tmp.k)
    pb.v(lambda: nc.vector.tensor_tensor(out=rstd.t[:], in0=ps2[:], in1=tmp.t[:], op=ALU.subtract), R=[k2, tmp.k[0]], W=rstd.k)
    pb.actf(rstd.t[:], rstd.t[:], AF.Sqrt, R=rstd.k, W=rstd.k, bias=c.eps.t[:, 0:1], scale=1.0)
    pb.v(lambda: nc.vector.reciprocal(out=rstd.t[:], in_=rstd.t[:]), R=rstd.k, W=rstd.k)
    for k in range(NCH):
        t2 = sc["t2"][k % 2]
        pb.v(lambda: nc.vector.tensor_tensor(out=t2.t[:], in0=r.t[:, k, :], in1=mean.t[:], op=ALU.subtract),
             R=[r.k[k], mean.k[0]], W=t2.k)
        pb.v(lambda: nc.vector.tensor_tensor(out=t2.t[:], in0=t2.t[:], in1=rstd.t[:], op=ALU.mult),
             R=[t2.k[0], rstd.k[0]], W=t2.k)
        pb.actf(r.t[:, k, :], t2.t[:], AF.Identity, R=[t2.k[0], c.lng.k[0], c.lnb.k[0]], W=[r.k[k]],
                scale=c.lng.t[:, li, k:k + 1], bias=c.lnb.t[:, li, k:k + 1])
        if out_bf is not None:
            pb.actf(out_bf.t[:, k, :], r.t[:, k, :], AF.Copy, R=[r.k[k]], W=[out_bf.k[k]])


def ln_scratch(pb, stack):
    sc = {}
    sc["sq"] = [Buf(pb, stack, "ln_sq%d" % i, [128, T], F32) for i in range(2)]
    sc["t2"] = [Buf(pb, stack, "ln_t2%d" % i, [128, T], F32) for i in range(2)]
    sc["mean"] = Buf(pb, stack, "ln_mean", [128, T], F32)
    sc["rstd"] = Buf(pb, stack, "ln_rstd", [128, T], F32)
    sc["tmp"] = Buf(pb, stack, "ln_tmp", [128, T], F32)
    return sc


def setup_mem(pb, stack, c, io, l, tmpT, ring, stage=None, stage_k=None):
    nc = pb.nc
    kT = Buf(pb, stack, "kTmem%d" % l, [128, 4, 256], BF16, ntok=4)
    vm = Buf(pb, stack, "vmem%d" % l, [128, 2, 512], BF16, ntok=2)
    for mc in range(2):
        if stage is None:
            stage, stage_k = pb.mem_stage.t[:], pb.mem_stage.k
        pb.dma(pb.sp, stage, io["mem"][mc * 128:(mc + 1) * 128, :], W=stage_k)
        for k in range(NCH):
            p, pk = pb.ps()
            pb.tr(p[:, 0:128], stage[:, k * 128:(k + 1) * 128], c.ident.t[:], R=list(stage_k) + [c.ident.k[0]], W=[pk])
            pb.v(lambda: nc.vector.tensor_copy(out=tmpT.t[:, k, mc * 128:(mc + 1) * 128], in_=p[:, 0:128]),
                 R=[pk], W=[tmpT.k[k]])
    if STOP <= 1.2:
        return kT, vm
    wv = wview(io["w_mem_kv"][l])
    sk = ring.load(wv[:, :, 0:512])
    if STOP <= 1.4:
        return kT, vm
    for h in range(4):
        p, pk = pb.ps()
        for k in range(NCH):
            pb.mm(p[:, 0:256], sk.t[:, k, h * 128:(h + 1) * 128], tmpT.t[:, k, 0:256], k == 0, k == NCH - 1,
                  R=[sk.k[0], tmpT.k[k]], W=[pk])
        pb.v(lambda: nc.vector.tensor_copy(out=kT.t[:, h, :], in_=p[:, 0:256]), R=[pk], W=[kT.k[h]])
    if STOP <= 1.6:
        return kT, vm
    sv = ring.load(wv[:, :, 512:1024])
    KVAR = int(os.environ.get("KVAR", "0"))
    if KVAR == 1:
        return kT, vm
    for mc in range(2):
        p, pk = pb.ps()
        for k in range(NCH):
            pb.mm(p[:], tmpT.t[:, k, mc * 128:(mc + 1) * 128], sv.t[:, k, :], k == 0, k == NCH - 1,
                  R=[sv.k[0], tmpT.k[k]], W=[pk])
        if KVAR == 2:
            continue
        if KVAR == 3:
            pb.v(lambda: nc.vector.tensor_copy(out=pb.mem_stage.t[:, 0:512], in_=p[:]), R=[pk], W=pb.mem_stage.k)
            continue
        if KVAR == 4:
            for hh in range(2):
                pb.v(lambda: nc.vector.tensor_copy(out=vm.t[:, mc, hh * 256:(hh + 1) * 256], in_=p[:, hh * 256:(hh + 1) * 256]), R=[pk], W=[vm.k[mc]])
            continue
        pb.v(lambda: nc.vector.tensor_copy(out=vm.t[:, mc, :], in_=p[:]), R=[pk], W=[vm.k[mc]])
    return kT, vm


def mem_attn(pb, c, kT, vm, qm, cat, pT, rc):
    nc = pb.nc
    sc = 128 ** -0.5
    for h in range(4):
        pts = []
        for mc in range(2):
            p, pk = pb.ps()
            pb.mm(p[:], kT.t[:, h, mc * 128:(mc + 1) * 128], qm.t[:, h, :], True, True, R=[kT.k[h], qm.k[h]], W=[pk])
            pt = pT[pb.pti % len(pT)]
            pb.pti += 1
            pb.actf(pt.t[:], p[:], AF.Exp, R=[pk], W=pt.k, scale=sc)
            pts.append(pt)
        po, pok = pb.ps()
        psm, psk = pb.ps()
        for mc in range(2):
            pb.mm(po[:], vm.t[:, mc, h * 128:(h + 1) * 128], pts[mc].t[:], mc == 0, mc == 1, R=[vm.k[mc], pts[mc].k[0]], W=[pok])
        for mc in range(2):
            pb.mm(psm[:], c.ones_b.t[:], pts[mc].t[:], mc == 0, mc == 1, R=[c.ones_b.k[0], pts[mc].k[0]], W=[psk])
        pb.v(lambda: nc.vector.reciprocal(out=rc.t[:], in_=psm[:]), R=[psk], W=rc.k)
        pb.v(lambda: nc.vector.tensor_tensor(out=cat.t[:, 12 + h, :], in0=po[:], in1=rc.t[:], op=ALU.mult),
             R=[pok, rc.k[0]], W=[cat.k[12 + h]])


def proj_fm(pb, ring_slab, xb, ncol_chunks, evac):
    for cc in range(ncol_chunks):
        p, pk = pb.ps()
        for k in range(NCH):
            pb.mm(p[:], ring_slab.t[:, k, cc * 128:(cc + 1) * 128], xb.t[:, k, :], k == 0, k == NCH - 1,
                  R=[ring_slab.k[0], xb.k[k]], W=[pk])
        evac(cc, p, pk)


def phase1(pb, c, io, ntiles):
    nc = pb.nc
    with ExitStack() as st:
        ring = Ring(pb, st, 4)
        xs = [Buf(pb, st, "xs%d" % i, [128, D], F32) for i in range(2)]
        xT = Buf(pb, st, "xT", [128, NCH, T], F32, ntok=NCH)
        xb = Buf(pb, st, "xb", [128, NCH, T], BF16, ntok=NCH)
        vt = Buf(pb, st, "vt", [128, NB, MAINW], BF16, ntok=NB)
        qm = Buf(pb, st, "qm", [128, 4, T], BF16, ntok=4)
        cat = Buf(pb, st, "cat", [128, NCH, T], BF16, ntok=NCH)
        pT = [Buf(pb, st, "pT%d" % i, [128, T], BF16) for i in range(4)]
        pb.pti = 0
        rc = Buf(pb, st, "rc", [128, T], F32)
        gt = [Buf(pb, st, "gt%d" % i, [128, T], F32) for i in range(2)]
        sc = ln_scratch(pb, st)
        bst = Buf(pb, st, "bst", [128, 3, 6], F32)
        mv = Buf(pb, st, "mv", [128, 2], F32)
        rs = Buf(pb, st, "rs", [128, 2], F32)
        pb.mem_stage = xs[0]
        wT = Buf(pb, st, "wT", [128, 12, 128], BF16)
        bias2 = Buf(pb, st, "bias2", [128, 12, 128], F32)
        vg = Buf(pb, st, "vg", [128, 12], F32)
        vb = Buf(pb, st, "vb", [128, 12], F32)
        with nc.allow_non_contiguous_dma(reason="tiny param loads"):
            pb.dma(pb.sp, vg.t[:], io["a_vnorm_g"][0].rearrange("(g d) -> d g", d=128), W=vg.k)
            pb.dma(pb.sp, vb.t[:], io["a_vnorm_b"][0].rearrange("(g d) -> d g", d=128), W=vb.k)
        pb.dma(pb.sp, bias2.t[:].rearrange("p g t -> p (g t)"),
               io["a_b_s"][0:1].rearrange("o g t -> o (g t)").partition_broadcast(128), W=bias2.k)
        wl = xT
        wlv = wl.t[:, 0:3, :].rearrange("p a (b s) -> p (a b) s", s=128)
        pb.dma(pb.sp, wlv, io["a_w_s"][0].rearrange("g t s -> t g s"), W=wl.k)
        wtmp = xT.t[:, 4:7, :].rearrange("p a (b s) -> p (a b) s", s=128)
        for g in range(12):
            p, pk = pb.ps()
            pb.tr(p[:, 0:128], wlv[:, g, :], c.ident.t[:], R=[wl.k[0], wl.k[1], wl.k[2], c.ident.k[0]], W=[pk])
            pb.v(lambda: nc.vector.tensor_copy(out=wtmp[:, g, :], in_=p[:, 0:128]), R=[pk], W=xT.k[4:7])
        pb.g(lambda: nc.gpsimd.affine_select(out=wT.t[:], in_=wtmp, pattern=[[0, 12], [1, 128]], compare_op=ALU.is_ge,
                                             fill=0.0, base=0, channel_multiplier=-1), R=xT.k[4:7], W=wT.k)
        for n in range(3):
            p, pk = pb.ps()
            pb.mm(p[:], c.ones_b.t[:], wT.t[:, n * 4:(n + 1) * 4, :], True, True, R=[c.ones_b.k[0], wT.k[0]], W=[pk])
            for gg in range(4):
                g = n * 4 + gg
                pb.v(lambda: nc.vector.scalar_tensor_tensor(out=bias2.t[:, g, :], in0=p[:, gg * 128:(gg + 1) * 128],
                                                            scalar=vb.t[:, g:g + 1], in1=bias2.t[:, g, :],
                                                            op0=ALU.mult, op1=ALU.add),
                     R=[pk, vb.k[0], bias2.k[0]], W=bias2.k)
        if STOP <= 1:
            pb.barrier(); return
        kTm, vmm = setup_mem(pb, st, c, io, 0, xb, ring)
        if STOP <= 2:
            pb.barrier(); return
        w_in = wview(io["a_w_in"][0])
        w_mix = wview(io["w_mix_out"][0])
        for lt in range(ntiles):
            _load_x_tile(pb, c, io, lt, xs, xT, xb)
            if STOP <= 3:
                pb.barrier(); return
            for n in range(3):
                s = ring.load(w_in[:, :, MAINW + n * 512: MAINW + (n + 1) * 512])
                for j in range(NB):
                    p, pk = pb.ps()
                    for k in range(NCH):
                        pb.mm(p[:], xb.t[:, k, j * 128:(j + 1) * 128], s.t[:, k, :], k == 0, k == NCH - 1,
                              R=[s.k[0], xb.k[k]], W=[pk])
                    pb.actf(vt.t[:, j, n * 512:(n + 1) * 512], p[:], GELU, R=[pk], W=[vt.k[j]])
            for j in range(NB):
                for n in range(3):
                    pb.v(lambda: nc.vector.bn_stats(out=bst.t[:, n, :], in_=vt.t[:, j, n * 512:(n + 1) * 512]),
                         R=[vt.k[j]], W=bst.k)
                pb.v(lambda: nc.vector.bn_aggr(out=mv.t[:], in_=bst.t[:].rearrange("p a b -> p (a b)")), R=bst.k, W=mv.k)
                pb.actf(rs.t[:, 0:1], mv.t[:, 1:2], AF.Sqrt, R=mv.k, W=rs.k, bias=c.eps.t[:, 0:1], scale=1.0)
                pb.v(lambda: nc.vector.reciprocal(out=rs.t[:, 0:1], in_=rs.t[:, 0:1]), R=rs.k, W=rs.k)
                pb.v(lambda: nc.vector.scalar_tensor_tensor(out=rs.t[:, 1:2], in0=mv.t[:, 0:1], scalar=-1.0, in1=rs.t[:, 0:1],
                                                            op0=ALU.mult, op1=ALU.mult), R=[mv.k[0], rs.k[0]], W=rs.k)
                pb.v(lambda: nc.vector.tensor_scalar(out=vt.t[:, j, :], in0=vt.t[:, j, :], scalar1=rs.t[:, 0:1],
                                                     scalar2=rs.t[:, 1:2], op0=ALU.mult, op1=ALU.add),
                     R=[vt.k[j], rs.k[0]], W=[vt.k[j]])
            if STOP <= 4:
                pb.barrier(); return
            for n in range(3):
                s = ring.load(w_in[:, :, n * 512:(n + 1) * 512])

                def evac_u(cc, p, pk, n=n):
                    g = n * 4 + cc
                    pb.actf(cat.t[:, g, :], p[:], GELU, R=[pk], W=[cat.k[g]])
                    p2, pk2 = pb.ps()
                    for j in range(NB):
                        pb.mm(p2[:, j * 128:(j + 1) * 128], vt.t[:, j, g * 128:(g + 1) * 128], wT.t[:, g, :], True, True,
                              R=[vt.k[j], wT.k[0]], W=[pk2], inc=(j == NB - 1))
                    t_ = gt[g % 2]
                    pb.v(lambda: nc.vector.scalar_tensor_tensor(
                        out=t_.t[:].rearrange("p (j t) -> p j t", t=128), in0=p2[:].rearrange("p (j t) -> p j t", t=128),
                        scalar=vg.t[:, g:g + 1], in1=bias2.t[:, g, :].unsqueeze(1).to_broadcast([128, NB, 128]),
                        op0=ALU.mult, op1=ALU.add), R=[pk2, vg.k[0], bias2.k[0]], W=t_.k)
                    pb.v(lambda: nc.vector.tensor_tensor(out=cat.t[:, g, :], in0=cat.t[:, g, :], in1=t_.t[:], op=ALU.mult),
                         R=[cat.k[g], t_.k[0]], W=[cat.k[g]])
                proj_fm(pb, s, xb, 4, evac_u)
            if STOP <= 5:
                pb.barrier(); return
            s = ring.load(w_in[:, :, 2 * MAINW: 2 * MAINW + 512])

            def evac_q(cc, p, pk):
                pb.v(lambda: nc.vector.tensor_copy(out=qm.t[:, cc, :], in_=p[:]), R=[pk], W=[qm.k[cc]])
            proj_fm(pb, s, xb, 4, evac_q)
            mem_attn(pb, c, kTm, vmm, qm, cat, pT, rc)
            if STOP <= 6:
                pb.barrier(); return
            for n in range(4):
                s = ring.load(w_mix[:, :, n * 512:(n + 1) * 512])

                def evac_m(cc, p, pk, n=n):
                    oc = n * 4 + cc
                    pb.v(lambda: nc.vector.scalar_tensor_tensor(out=xT.t[:, oc, :], in0=xT.t[:, oc, :], scalar=ALPHA, in1=p[:],
                                                                op0=ALU.mult, op1=ALU.add), R=[pk, xT.k[oc]], W=[xT.k[oc]])
                proj_fm(pb, s, cat, 4, evac_m)
            if STOP <= 7:
                pb.barrier(); return
            ln_fm(pb, c, sc, xT, 0)
            pb.dma(pb.sp, io["x1a"][:, :, lt * T:(lt + 1) * T].rearrange("k p t -> p k t"), xT.t[:], R=xT.k, W=[io["x1a_tok"][lt]])
    pb.barrier()


def _load_x_tile(pb, c, io, lt, xs, xT, xb):
    nc = pb.nc
    for j in range(NB):
        x_ = xs[j % 2]
        pb.dma(pb.sp, x_.t[:], io["x"][lt * T + j * 128: lt * T + (j + 1) * 128, :], W=x_.k)
        for k4 in range(4):
            p, pk = pb.ps()
            for kk in range(4):
                k = k4 * 4 + kk
                pb.tr(p[:, kk * 128:(kk + 1) * 128], x_.t[:, k * 128:(k + 1) * 128], c.ident.t[:],
                      R=[x_.k[0], c.ident.k[0]], W=[pk], inc=(kk == 3))
            toks = [xT.k[k4 * 4 + kk] for kk in range(4)]
            pb.v(lambda: nc.vector.tensor_copy(out=xT.t[:, k4 * 4:(k4 + 1) * 4, j * 128:(j + 1) * 128],
                                               in_=p[:].rearrange("p (a t) -> p a t", t=128)), R=[pk], W=toks)
            btoks = [xb.k[k4 * 4 + kk] for kk in range(4)]
            pb.actf(xb.t[:, k4 * 4:(k4 + 1) * 4, j * 128:(j + 1) * 128], xT.t[:, k4 * 4:(k4 + 1) * 4, j * 128:(j + 1) * 128],
                    AF.Copy, R=toks, W=btoks)


def dram_in(nc, name, shape, dt=F32):
    return nc.dram_tensor(name, list(shape), dt, kind="ExternalInput").ap()


def dram_out(nc, name, shape, dt=F32):
    return nc.dram_tensor(name, list(shape), dt, kind="ExternalOutput").ap()


def load_fm_tile(pb, src3, lt, xT, xb, rtok):
    v = src3[:, :, lt * T:(lt + 1) * T].rearrange("k p t -> p k t")
    pb.dma(pb.sp, xT.t[:], v, R=[rtok], W=xT.k)
    if xb is not None:
        pb.dma(pb.pool, xb.t[:], v, R=[rtok], W=xb.k)


def down_proj(pb, ring, wd3, nk, kslab, hid, k0, evac):
    nq = nk // kslab
    for og in range(4):
        acc = [pb.ps() for _ in range(4)]
        for q in range(nq):
            s = ring.load(wd3[:, k0 + q * kslab: k0 + (q + 1) * kslab, og * 512:(og + 1) * 512], kc=kslab)
            for oc in range(4):
                for kk in range(kslab):
                    kl = q * kslab + kk
                    pb.mm(acc[oc][0][:], s.t[:, kk, oc * 128:(oc + 1) * 128], hid.t[:, kl, :],
                          q == 0 and kk == 0, q == nq - 1 and kk == kslab - 1,
                          R=[s.k[0], hid.k[kl]], W=[acc[oc][1]], inc=(kk == kslab - 1 and (oc == 3 or q == nq - 1)))
        for oc in range(4):
            evac(og * 4 + oc, acc[oc][0], acc[oc][1])


def gate_up(pb, ring, wg3, wu3, c0, nslab, xb, hid, sg):
    nc = pb.nc
    for n in range(nslab):
        col = (c0 + n * 4) * 128
        sgt = ring.load(wg3[:, :, col: col + 512])
        sup = ring.load(wu3[:, :, col: col + 512])
        for cc in range(4):
            pg, pgk = pb.ps()
            pu, puk = pb.ps()
            for k in range(NCH):
                pb.mm(pg[:], sgt.t[:, k, cc * 128:(cc + 1) * 128], xb.t[:, k, :], k == 0, k == NCH - 1,
                      R=[sgt.k[0], xb.k[k]], W=[pgk])
            for k in range(NCH):
                pb.mm(pu[:], sup.t[:, k, cc * 128:(cc + 1) * 128], xb.t[:, k, :], k == 0, k == NCH - 1,
                      R=[sup.k[0], xb.k[k]], W=[puk])
            g_ = sg[(n * 4 + cc) % len(sg)]
            pb.actf(g_.t[:], pg[:], AF.Silu, R=[pgk], W=g_.k)
            i = n * 4 + cc
            pb.v(lambda: nc.vector.tensor_tensor(out=hid.t[:, i, :], in0=g_.t[:], in1=pu[:], op=ALU.mult),
                 R=[g_.k[0], puk], W=[hid.k[i]])


def phase2(pb, c, io, ntiles):
    nc = pb.nc
    NFC = DFF // 128
    with ExitStack() as st:
        ring = Ring(pb, st, 4)
        xT = Buf(pb, st, "xT2", [128, NCH, T], F32, ntok=NCH)
        xb = Buf(pb, st, "xb2", [128, NCH, T], BF16, ntok=NCH)
        hid = Buf(pb, st, "hid2", [128, NFC, T], BF16, ntok=NFC)
        sg = [Buf(pb, st, "sg%d" % i, [128, T], F32) for i in range(2)]
        kst = Buf(pb, st, "kst", [128, 12, T], BF16, ntok=12)
        vst = Buf(pb, st, "vst", [128, NB, MAINW], BF16, ntok=NB)
        sc = ln_scratch(pb, st)
        wg3 = wview(io["ffn_w_gate"][0])
        wu3 = wview(io["ffn_w_up"][0])
        wd3 = wview(io["ffn_w_down"][0])
        wkv = wview(io["shared_w_kv"])
        for lt in range(ntiles):
            load_fm_tile(pb, io["x1a"], lt, xT, xb, io["x1a_tok"][lt])
            gate_up(pb, ring, wg3, wu3, 0, NFC // 4, xb, hid, sg)

            def evac_d(oc, p, pk):
                pb.v(lambda: nc.vector.scalar_tensor_tensor(out=xT.t[:, oc, :], in0=xT.t[:, oc, :], scalar=ALPHA, in1=p[:],
                                                            op0=ALU.mult, op1=ALU.add), R=[pk, xT.k[oc]], W=[xT.k[oc]])
            down_proj(pb, ring, wd3, NFC, 11, hid, 0, evac_d)
            ln_fm(pb, c, sc, xT, 1, out_bf=xb)
            pb.dma(pb.sp, io["x1"][:, :, lt * T:(lt + 1) * T].rearrange("k p t -> p k t"), xT.t[:], R=xT.k, W=[io["x1_tok"][lt]])
            for n in range(3):
                s = ring.load(wkv[:, :, n * 512:(n + 1) * 512])

                def evac_k(cc, p, pk, n=n):
                    ci = n * 4 + cc
                    pb.actf(kst.t[:, ci, :], p[:], AF.Copy, R=[pk], W=[kst.k[ci]])
                proj_fm(pb, s, xb, 4, evac_k)
            pb.dma(pb.sp, io["kT"][:, :, lt * T:(lt + 1) * T].rearrange("c p t -> p c t"), kst.t[:], R=kst.k, W=[io["kv_tok"][lt]])
            for n in range(3):
                s = ring.load(wkv[:, :, MAINW + n * 512: MAINW + (n + 1) * 512])
                for j in range(NB):
                    p, pk = pb.ps()
                    for k in range(NCH):
                        pb.mm(p[:], xb.t[:, k, j * 128:(j + 1) * 128], s.t[:, k, :], k == 0, k == NCH - 1,
                              R=[s.k[0], xb.k[k]], W=[pk])
                    pb.v(lambda: nc.vector.tensor_copy(out=vst.t[:, j, n * 512:(n + 1) * 512], in_=p[:]), R=[pk], W=[vst.k[j]])
            pb.dma(pb.sp, io["v"][lt * NB:(lt + 1) * NB].rearrange("j p c -> p j c"), vst.t[:], R=vst.k, W=[io["kv_tok"][lt]])
    pb.barrier()


A_PARAMS = [("mem", [256, D]), ("ln_g", [2, 2, D]), ("ln_b", [2, 2, D]), ("w_mix_out", [2, D, D]), ("w_mem_kv", [2, D, 1024]),
            ("a_w_in", [1, D, 3584]), ("a_vnorm_g", [1, MAINW]), ("a_vnorm_b", [1, MAINW]), ("a_w_s", [1, 12, 128, 128]),
            ("a_b_s", [1, 12, 128]), ("shared_w_kv", [D, 3072]), ("ffn_w_gate", [1, D, DFF]), ("ffn_w_up", [1, D, DFF]),
            ("ffn_w_down", [1, DFF, D])]


def build_A(ntiles=NT_CORE):
    nc = bass.Bass("TRN2", target_bir_lowering=False)
    ntok = ntiles * T
    io = {"x": dram_in(nc, "x", [ntok, D])}
    for n, shp in A_PARAMS:
        io[n] = dram_in(nc, n, shp)
    io["x1a"] = nc.dram_tensor("x1a", [NCH, 128, ntok], F32, kind="Internal").ap()
    io["x1"] = dram_out(nc, "x1", [NCH, 128, ntok])
    io["kT"] = dram_out(nc, "kT", [12, 128, ntok], BF16)
    io["v"] = dram_out(nc, "v", [ntiles * NB, 128, MAINW], BF16)
    for n in ("x1a_tok", "x1_tok", "kv_tok"):
        io[n] = [Tok() for _ in range(ntiles)]
    pb = PB(nc)
    with ExitStack() as st:
        c = setup_consts(pb, st, io)
        phase1(pb, c, io, ntiles)
        phase2(pb, c, io, ntiles)
        pb.finish()
    return nc


LAMBDA_INIT = 0.8 - 0.6 * math.exp(-0.3 * 1)


def phase3(pb, c, io, ntiles):
    nc = pb.nc
    scale = 128 ** -0.5
    with ExitStack() as st:
        ring = Ring(pb, st, 3)
        xT = Buf(pb, st, "xT3", [128, NCH, T], F32, ntok=NCH)
        xb = Buf(pb, st, "xb3", [128, NCH, T], BF16, ntok=NCH)
        QT = Buf(pb, st, "QT", [128, 12, T], BF16, ntok=12)
        qm = Buf(pb, st, "qm3", [128, 4, T], BF16, ntok=4)
        cat = Buf(pb, st, "cat3", [128, NCH, T], BF16, ntok=NCH)
        pT = [Buf(pb, st, "pT3_%d" % i, [128, T], BF16) for i in range(6)]
        pb.pti = 0
        kvK = [Buf(pb, st, "kvK%d" % i, [128, 2, T], BF16) for i in range(4)]
        kvV = [Buf(pb, st, "kvV%d" % i, [128, NB, 256], BF16) for i in range(4)]
        kvi = [0]
        qpos = Buf(pb, st, "qpos", [128, T], F32)
        kpos = {"own": Buf(pb, st, "kpo", [128, 32], F32), "par": Buf(pb, st, "kpp", [128, 32], F32)}
        msk = [Buf(pb, st, "msk%d" % i, [128, T], BF16) for i in range(2)]
        mski = [0]
        rc0 = Buf(pb, st, "rc0", [128, T], F32)
        rc1 = Buf(pb, st, "rc1", [128, T], F32)
        tt = Buf(pb, st, "tt", [128, T], F32)
        ao = [Buf(pb, st, "ao%d" % i, [128, T], F32) for i in range(2)]
        sq = [Buf(pb, st, "sq%d" % i, [128, T], BF16) for i in range(2)]
        rs_ = Buf(pb, st, "rs3", [128, T], F32)
        sc = ln_scratch(pb, st)
        lq = Buf(pb, st, "lq", [128, 4, 128], F32)
        lsm = Buf(pb, st, "lsm", [128, 4], F32)
        sgc = Buf(pb, st, "sgc", [128, 2], F32)
        for i, n in enumerate(("b_lambda_q1", "b_lambda_k1", "b_lambda_q2", "b_lambda_k2")):
            pb.dma(pb.sp, lq.t[:, i, :], io[n][0:1, :].partition_broadcast(128), W=lq.k)
        with nc.allow_non_contiguous_dma(reason="tiny param loads"):
            pb.dma(pb.sp, sgc.t[:], io["b_subln_g"][0].rearrange("(c p) -> p c", p=128), W=sgc.k)
        pb.dma(pb.sp, kpos["own"].t[:], io["kpos_own"], W=kpos["own"].k)
        pb.dma(pb.sp, kpos["par"].t[:], io["kpos_par"], W=kpos["par"].k)
        pb.v(lambda: nc.vector.tensor_scalar(out=sgc.t[:], in0=sgc.t[:], scalar1=1.0 - LAMBDA_INIT, scalar2=None, op0=ALU.mult),
             R=sgc.k, W=sgc.k)
        for i in range(2):
            pb.v(lambda: nc.vector.tensor_tensor(out=lq.t[:, 2 * i, :], in0=lq.t[:, 2 * i, :], in1=lq.t[:, 2 * i + 1, :], op=ALU.mult),
                 R=lq.k, W=lq.k)
            pb.v(lambda: nc.vector.reduce_sum(out=lsm.t[:, i:i + 1], in_=lq.t[:, 2 * i, :], axis=mybir.AxisListType.X), R=lq.k, W=lsm.k)
        pb.actf(lsm.t[:, 0:2], lsm.t[:, 0:2], AF.Exp, R=lsm.k, W=lsm.k)
        pb.v(lambda: nc.vector.tensor_tensor(out=lsm.t[:, 2:3], in0=lsm.t[:, 1:2], in1=lsm.t[:, 0:1], op=ALU.subtract), R=lsm.k, W=lsm.k)
        pb.v(lambda: nc.vector.tensor_scalar(out=lsm.t[:, 2:3], in0=lsm.t[:, 2:3], scalar1=-LAMBDA_INIT, scalar2=None, op0=ALU.add),
             R=lsm.k, W=lsm.k)
        kTm, vmm = setup_mem(pb, st, c, io, 1, xb, ring, stage=xT.t[:, 0:4, :].rearrange("p a t -> p (a t)"), stage_k=xT.k[0:4])
        w_in = wview(io["b_w_in"][0])
        w_mix = wview(io["w_mix_out"][1])
        Ob = [(pb.banks[i], pb.btok[i]) for i in range(4)]
        Sm = [(pb.banks[4], pb.btok[4]), (pb.banks[5], pb.btok[5])]
        Sc = [(pb.banks[6], pb.btok[6]), (pb.banks[7], pb.btok[7])]

        def attention(lt, h):
            klist = [("par", kt) for kt in range(lt + 1)] + [("own", kt) for kt in range(lt + 1)]
            pieces = []
            for (src, kt) in klist:
                i = kvi[0] % 4
                kvi[0] += 1
                pieces.append((src, kt, kvK[i], kvV[i]))

            def load_piece(pi):
                src, kt, kb, vb = pieces[pi]
                for m in range(2):
                    pb.dma(pb.sp, kb.t[:, m, :], io["kT_" + src][m * 6 + h][:, kt * T:(kt + 1) * T], W=kb.k)
                pb.dma(pb.sp, vb.t[:], io["v_" + src][kt * NB:(kt + 1) * NB, :, h * 256:(h + 1) * 256].rearrange("j p c -> p j c"),
                       W=vb.k)
            steps = []
            for pi, (src, kt, kb, vb) in enumerate(pieces):
                for jb in range(NB):
                    diag = (src == "own" and kt == lt)
                    masked = diag or (src == "par" and kt == lt)
                    steps.append((kb, vb, jb, masked, kpos[src].t[:, kt * NB + jb: kt * NB + jb + 1], kpos[src].k[0],
                                  jb * 128 if diag else 0))
            for pi in range(min(3, len(pieces))):
                load_piece(pi)
            nst = len(steps)

            def pv(idx, stp, pts):
                kb, vb, jb, masked, kcol, kck, c0 = stp
                first, last = idx == 0, idx == nst - 1
                for m in range(2):
                    for dvc in range(2):
                        o, ok = Ob[m * 2 + dvc]
                        pb.mm(o[:, c0:T], vb.t[:, jb, dvc * 128:(dvc + 1) * 128], pts[m].t[:, c0:T], first, last,
                              R=[vb.k[0], pts[m].k[0]], W=[ok], inc=False)
                    o, ok = Sm[m]
                    pb.mm(o[:, c0:T], c.ones_b.t[:], pts[m].t[:, c0:T], first, last, R=[c.ones_b.k[0], pts[m].k[0]], W=[ok],
                          inc=(m == 1))
            prev = None
            for idx, stp in enumerate(steps):
                kb, vb, jb, masked, kcol, kck, c0 = stp
                pts = []
                if masked:
                    mk_ = msk[mski[0] % 2]
                    mski[0] += 1
                    pb.v(lambda: nc.vector.tensor_scalar(out=mk_.t[:, c0:T], in0=qpos.t[:, c0:T], scalar1=kcol, scalar2=None,
                                                         op0=ALU.is_ge), R=[qpos.k[0], kck], W=mk_.k)
                for m in range(2):
                    sb, sbk = Sc[m]
                    pb.mm(sb[:, c0:T], kb.t[:, m, jb * 128:(jb + 1) * 128], QT.t[:, m * 6 + h, c0:T], True, True,
                          R=[kb.k[0], QT.k[m * 6 + h]], W=[sbk], inc=True)
                    pt = pT[pb.pti % len(pT)]
                    pb.pti += 1
                    pb.actf(pt.t[:, c0:T], sb[:, c0:T], AF.Exp, R=[sbk], W=pt.k, scale=scale)
                    if masked:
                        pb.v(lambda: nc.vector.tensor_tensor(out=pt.t[:, c0:T], in0=pt.t[:, c0:T], in1=mk_.t[:, c0:T], op=ALU.mult),
                             R=[pt.k[0], mk_.k[0]], W=pt.k)
                    pts.append(pt)
                if prev is not None:
                    pv(*prev)
                prev = (idx, stp, pts)
                if idx % NB == NB - 1 and idx // NB + 3 < len(pieces):
                    load_piece(idx // NB + 3)
            pv(*prev)
            pb.v(lambda: nc.vector.reciprocal(out=rc0.t[:], in_=Sm[0][0][:]), R=[Sm[0][1]], W=rc0.k)
            pb.v(lambda: nc.vector.reciprocal(out=rc1.t[:], in_=Sm[1][0][:]), R=[Sm[1][1]], W=rc1.k)
            pb.v(lambda: nc.vector.tensor_scalar(out=rc1.t[:], in0=rc1.t[:], scalar1=lsm.t[:, 2:3], scalar2=None, op0=ALU.mult),
                 R=[rc1.k[0], lsm.k[0]], W=rc1.k)
            for dvc in range(2):
                a_ = ao[dvc]
                pb.v(lambda: nc.vector.tensor_tensor(out=tt.t[:], in0=Ob[dvc][0][:], in1=rc0.t[:], op=ALU.mult),
                     R=[Ob[dvc][1], rc0.k[0]], W=tt.k)
                pb.v(lambda: nc.vector.tensor_tensor(out=a_.t[:], in0=Ob[2 + dvc][0][:], in1=rc1.t[:], op=ALU.mult),
                     R=[Ob[2 + dvc][1], rc1.k[0]], W=a_.k)
                pb.v(lambda: nc.vector.tensor_tensor(out=a_.t[:], in0=a_.t[:], in1=tt.t[:], op=ALU.add), R=[a_.k[0], tt.k[0]], W=a_.k)
                pb.actf(sq[dvc].t[:], a_.t[:], AF.Square, R=a_.k, W=sq[dvc].k)
            ssb, ssk = Sc[0]
            for dvc in range(2):
                pb.mm(ssb[:], c.ones_b.t[:], sq[dvc].t[:], dvc == 0, dvc == 1, R=[c.ones_b.k[0], sq[dvc].k[0]], W=[ssk])
            pb.actf(rs_.t[:], ssb[:], AF.Sqrt, R=[ssk], W=rs_.k, scale=1.0 / 256, bias=c.eps.t[:, 0:1])
            pb.v(lambda: nc.vector.reciprocal(out=rs_.t[:], in_=rs_.t[:]), R=rs_.k, W=rs_.k)
            for dvc in range(2):
                a_ = ao[dvc]
                pb.v(lambda: nc.vector.tensor_tensor(out=a_.t[:], in0=a_.t[:], in1=rs_.t[:], op=ALU.mult), R=[a_.k[0], rs_.k[0]], W=a_.k)
                pb.actf(cat.t[:, h * 2 + dvc, :], a_.t[:], AF.Identity, R=[a_.k[0], sgc.k[0]], W=[cat.k[h * 2 + dvc]],
                        scale=sgc.t[:, dvc:dvc + 1])

        for lt in range(ntiles):
            load_fm_tile(pb, io["x1"], lt, xT, xb, io["x1_tok"][lt])
            pb.dma(pb.sp, qpos.t[:], io["qpos"][0:1, lt * T:(lt + 1) * T].partition_broadcast(128), W=qpos.k)
            for n in range(4):
                s = ring.load(w_in[:, :, n * 512:(n + 1) * 512])

                def evac_q(cc, p, pk, n=n):
                    ci = n * 4 + cc
                    if ci < 12:
                        pb.actf(QT.t[:, ci, :], p[:], AF.Copy, R=[pk], W=[QT.k[ci]])
                    else:
                        pb.actf(qm.t[:, ci - 12, :], p[:], AF.Copy, R=[pk], W=[qm.k[ci - 12]])
                proj_fm(pb, s, xb, 4, evac_q)
            for h in range(6):
                attention(lt, h)
            mem_attn(pb, c, kTm, vmm, qm, cat, pT, rc0)
            for n in range(4):
                s = ring.load(w_mix[:, :, n * 512:(n + 1) * 512])

                def evac_m(cc, p, pk, n=n):
                    oc = n * 4 + cc
                    pb.v(lambda: nc.vector.scalar_tensor_tensor(out=xT.t[:, oc, :], in0=xT.t[:, oc, :], scalar=ALPHA, in1=p[:],
                                                                op0=ALU.mult, op1=ALU.add), R=[pk, xT.k[oc]], W=[xT.k[oc]])
                proj_fm(pb, s, cat, 4, evac_m)
            ln_fm(pb, c, sc, xT, 2)
            pb.dma(pb.sp, io["x2"][:, :, lt * T:(lt + 1) * T].rearrange("k p t -> p k t"), xT.t[:], R=xT.k, W=[io["x2_tok"][lt]])
    pb.barrier()


def phase4(pb, c, io, ntiles):
    nc = pb.nc
    NEC = DFE // 128
    HC = NEC // 2
    with ExitStack() as st:
        ring = Ring(pb, st, 4)
        xT = Buf(pb, st, "xT4", [128, NCH, T], F32, ntok=NCH)
        xb = Buf(pb, st, "xb4", [128, NCH, T], BF16, ntok=NCH)
        hid = Buf(pb, st, "hid4", [128, HC, T], BF16, ntok=HC)
        sg = [Buf(pb, st, "sg4_%d" % i, [128, T], F32) for i in range(2)]
        gb = [Buf(pb, st, "gb%d" % e, [128, T], F32) for e in range(NE)]
        tmp = [Buf(pb, st, "tmp4_%d" % i, [128, T], F32) for i in range(2)]
        ost = [Buf(pb, st, "ost%d" % i, [128, D], F32) for i in range(2)]
        sc = ln_scratch(pb, st)
        wr = Buf(pb, st, "wr", [128, NCH, NE], F32)
        lg = Buf(pb, st, "lg", [8, T], F32)
        lgt = Buf(pb, st, "lgt", [128, NB, NE], F32)
        m1 = Buf(pb, st, "m1", [128, NB], F32)
        m2 = Buf(pb, st, "m2", [128, NB], F32)
        eq = Buf(pb, st, "eq", [128, NB, NE], F32)
        l2 = Buf(pb, st, "l2", [128, NB, NE], F32)
        ex = Buf(pb, st, "ex", [128, NB, NE], F32)
        den = Buf(pb, st, "den", [128, NB], F32)
        grep_ = [Buf(pb, st, "grep%d" % i, [128, 128], F32) for i in range(2)]
        with nc.allow_non_contiguous_dma(reason="tiny param loads"):
            pb.dma(pb.sp, wr.t[:], io["moe_w_router"][0].rearrange("(k p) e -> p k e", p=128), W=wr.k)
        bc = lambda b: b.t[:].unsqueeze(2).to_broadcast([128, NB, NE])
        ti = [0]
        for lt in range(ntiles):
            load_fm_tile(pb, io["x2"], lt, xT, xb, io["x2_tok"][lt])
            p, pk = pb.ps()
            for k in range(NCH):
                pb.mm(p[0:8, :], wr.t[:, k, :], xT.t[:, k, :], k == 0, k == NCH - 1, R=[wr.k[0], xT.k[k]], W=[pk])
            pb.v(lambda: nc.vector.tensor_copy(out=lg.t[:], in_=p[0:8, :]), R=[pk], W=lg.k)
            p2, pk2 = pb.ps()
            for j in range(NB):
                pb.tr(p2[:, j * NE:(j + 1) * NE], lg.t[0:8, j * 128:(j + 1) * 128], c.ident.t[0:8, 0:8], R=[lg.k[0], c.ident.k[0]],
                      W=[pk2], inc=(j == NB - 1))
            pb.v(lambda: nc.vector.tensor_copy(out=lgt.t[:], in_=p2[:, 0:NB * NE].rearrange("p (j e) -> p j e", e=NE)), R=[pk2], W=lgt.k)
            pb.v(lambda: nc.vector.reduce_max(out=m1.t[:], in_=lgt.t[:], axis=mybir.AxisListType.X), R=lgt.k, W=m1.k)
            pb.v(lambda: nc.vector.tensor_tensor(out=eq.t[:], in0=lgt.t[:], in1=bc(m1), op=ALU.is_ge), R=[lgt.k[0], m1.k[0]], W=eq.k)
            pb.v(lambda: nc.vector.scalar_tensor_tensor(out=l2.t[:], in0=eq.t[:], scalar=-1e30, in1=lgt.t[:], op0=ALU.mult, op1=ALU.add),
                 R=[eq.k[0], lgt.k[0]], W=l2.k)
            pb.v(lambda: nc.vector.reduce_max(out=m2.t[:], in_=l2.t[:], axis=mybir.AxisListType.X), R=l2.k, W=m2.k)
            pb.v(lambda: nc.vector.tensor_tensor(out=eq.t[:], in0=lgt.t[:], in1=bc(m2), op=ALU.is_ge), R=[lgt.k[0], m2.k[0]], W=eq.k)
            pb.v(lambda: nc.vector.tensor_tensor(out=l2.t[:], in0=lgt.t[:], in1=bc(m1), op=ALU.subtract), R=[lgt.k[0], m1.k[0]], W=l2.k)
            pb.actf(ex.t[:], l2.t[:], AF.Exp, R=l2.k, W=ex.k)
            pb.v(lambda: nc.vector.tensor_tensor(out=ex.t[:], in0=ex.t[:], in1=eq.t[:], op=ALU.mult), R=[ex.k[0], eq.k[0]], W=ex.k)
            pb.v(lambda: nc.vector.reduce_sum(out=den.t[:], in_=ex.t[:], axis=mybir.AxisListType.X), R=ex.k, W=den.k)
            pb.v(lambda: nc.vector.reciprocal(out=den.t[:], in_=den.t[:]), R=den.k, W=den.k)
            pb.v(lambda: nc.vector.tensor_tensor(out=ex.t[:], in0=ex.t[:], in1=bc(den), op=ALU.mult), R=[ex.k[0], den.k[0]], W=ex.k)
            for e in range(NE):
                pg, pgk = pb.ps()
                for j in range(NB):
                    gr = grep_[(e * NB + j) % 2]
                    pb.v(lambda: nc.vector.tensor_copy(out=gr.t[:], in_=ex.t[:, j, e:e + 1].to_broadcast([128, 128])), R=ex.k, W=gr.k)
                    pb.mm(pg[:, j * 128:(j + 1) * 128], gr.t[:], c.ident.t[:], True, True, R=[gr.k[0], c.ident.k[0]], W=[pgk], inc=True)
                pb.v(lambda: nc.vector.tensor_copy(out=gb[e].t[:], in_=pg[:]), R=[pgk], W=gb[e].k)
            for k in range(NCH):
                pb.v(lambda: nc.vector.tensor_scalar(out=xT.t[:, k, :], in0=xT.t[:, k, :], scalar1=ALPHA, scalar2=None, op0=ALU.mult),
                     R=[xT.k[k]], W=[xT.k[k]])
            for e in range(NE):
                wg3 = wview(io["moe_w_gate"][0, e])
                wu3 = wview(io["moe_w_up"][0, e])
                wd3 = wview(io["moe_w_down"][0, e])
                for half in range(2):
                    gate_up(pb, ring, wg3, wu3, half * HC, HC // 4, xb, hid, sg)

                    def evac_e(oc, p, pk, e=e):
                        t_ = tmp[ti[0] % 2]
                        ti[0] += 1
                        pb.v(lambda: nc.vector.tensor_tensor(out=t_.t[:], in0=p[:], in1=gb[e].t[:], op=ALU.mult),
                             R=[pk, gb[e].k[0]], W=t_.k)
                        pb.v(lambda: nc.vector.tensor_tensor(out=xT.t[:, oc, :], in0=xT.t[:, oc, :], in1=t_.t[:], op=ALU.add),
                             R=[xT.k[oc], t_.k[0]], W=[xT.k[oc]])
                    down_proj(pb, ring, wd3, HC, 14, hid, half * HC, evac_e)
            ln_fm(pb, c, sc, xT, 3)
            for j in range(NB):
                o_ = ost[j % 2]
                for k4 in range(4):
                    p, pk = pb.ps()
                    for kk in range(4):
                        k = k4 * 4 + kk
                        pb.tr(p[:, kk * 128:(kk + 1) * 128], xT.t[:, k, j * 128:(j + 1) * 128], c.ident.t[:],
                              R=[xT.k[k], c.ident.k[0]], W=[pk], inc=(kk == 3))
                    pb.v(lambda: nc.vector.tensor_copy(out=o_.t[:, k4 * 512:(k4 + 1) * 512], in_=p[:]), R=[pk], W=o_.k)
                pb.dma(pb.sp, io["out"][lt * T + j * 128: lt * T + (j + 1) * 128, :], o_.t[:], R=o_.k, W=[io["out_tok"]])
    pb.barrier()


B_PARAMS = [("mem", [256, D]), ("ln_g", [2, 2, D]), ("ln_b", [2, 2, D]), ("w_mix_out", [2, D, D]), ("w_mem_kv", [2, D, 1024]),
            ("b_w_in", [1, D, D]), ("b_lambda_q1", [1, 128]), ("b_lambda_k1", [1, 128]), ("b_lambda_q2", [1, 128]),
            ("b_lambda_k2", [1, 128]), ("b_subln_g", [1, 256]), ("moe_w_router", [1, D, NE]),
            ("moe_w_gate", [1, NE, D, DFE]), ("moe_w_up", [1, NE, D, DFE]), ("moe_w_down", [1, NE, DFE, D])]


def build_B(ntiles=NT_CORE):
    nc = bass.Bass("TRN2", target_bir_lowering=False)
    ntok = ntiles * T
    io = {}
    for n, shp in B_PARAMS:
        io[n] = dram_in(nc, n, shp)
    io["x1"] = dram_in(nc, "x1", [NCH, 128, ntok])
    for sfx in ("own", "par"):
        io["kT_" + sfx] = dram_in(nc, "kT_" + sfx, [12, 128, ntok], BF16)
        io["v_" + sfx] = dram_in(nc, "v_" + sfx, [ntiles * NB, 128, MAINW], BF16)
        io["kpos_" + sfx] = dram_in(nc, "kpos_" + sfx, [128, 32])
    io["qpos"] = dram_in(nc, "qpos", [1, ntok])
    io["x2"] = nc.dram_tensor("x2", [NCH, 128, ntok], F32, kind="Internal").ap()
    io["out"] = dram_out(nc, "out", [ntok, D])
    io["x1_tok"] = [Tok() for _ in range(ntiles)]
    io["x2_tok"] = [Tok() for _ in range(ntiles)]
    io["out_tok"] = Tok()
    pb = PB(nc)
    with ExitStack() as st:
        c = setup_consts(pb, st, io)
        phase3(pb, c, io, ntiles)
        phase4(pb, c, io, ntiles)
        pb.finish()
    return nc


_CACHE = {}


def _core_tokens(r):
    return np.concatenate([np.arange(g * T, (g + 1) * T) for g in GT[r]])


def kernel(**inputs):
    n = 8
    x = np.asarray(inputs["x"])
    mem = np.asarray(inputs["mem"])
    if "A" not in _CACHE:
        _CACHE["A"] = build_A()
    ncA = _CACHE["A"]
    in_maps = []
    for cid in range(n):
        b, r = cid // 2, cid % 2
        m = {"x": np.ascontiguousarray(x[b][_core_tokens(r)]), "mem": np.ascontiguousarray(mem[b])}
        for nm, _ in A_PARAMS:
            if nm != "mem":
                m[nm] = np.asarray(inputs[nm])
        in_maps.append(m)
    resA = run_bass_kernel_spmd(ncA, in_maps, core_ids=list(range(n))).results
    if "B" not in _CACHE:
        _CACHE["B"] = build_B()
    ncB = _CACHE["B"]
    in_maps = []
    for cid in range(n):
        b, r = cid // 2, cid % 2
        par = cid ^ 1
        m = {"mem": np.ascontiguousarray(mem[b])}
        for nm, _ in B_PARAMS:
            if nm != "mem":
                m[nm] = np.asarray(inputs[nm])
        m["x1"] = resA[cid]["x1"]
        m["kT_own"], m["v_own"] = resA[cid]["kT"], resA[cid]["v"]
        m["kT_par"], m["v_par"] = resA[par]["kT"], resA[par]["v"]
        m["qpos"] = _core_tokens(r).astype(np.float32)[None, :]
        m["kpos_own"] = np.ascontiguousarray(_core_tokens(r).astype(np.float32).reshape(32, 128).T)
        m["kpos_par"] = np.ascontiguousarray(_core_tokens(1 - r).astype(np.float32).reshape(32, 128).T)
        in_maps.append(m)
    resB = run_bass_kernel_spmd(ncB, in_maps, core_ids=list(range(n))).results
    out = np.empty((4, SEQ, D), np.float32)
    for cid in range(n):
        b, r = cid // 2, cid % 2
        out[b][_core_tokens(r)] = resB[cid]["out"]
    return out
```
